# Optimizing a Trainium2 kernel written in Bass

```python
import math
import jax, jax.numpy as jnp
from jax import lax
import numpy as np

D_MODEL = 1024
BATCH = 8
SEQ = 8192
DEPTH = 1

DIFF_HEADS = 4
DIFF_HEAD_DIM = 64
DIFF_VAL_DIM = 2 * DIFF_HEAD_DIM
RET_HEADS = 4
RET_KEY_DIM = 64
RET_VAL_DIM = 128
D_FF = 256 * ((8 * D_MODEL // 3 + 255) // 256)
CONV_WIDTH = 3
Q_BLOCK = 128
RET_CHUNK = 128
NORM_EPS = 1e-6
SUBLN_EPS = 1e-5

SPLIT_SIZES = (
    DIFF_HEADS * DIFF_HEAD_DIM,
    DIFF_HEADS * DIFF_HEAD_DIM,
    DIFF_HEADS * DIFF_HEAD_DIM,
    DIFF_HEADS * DIFF_HEAD_DIM,
    DIFF_HEADS * DIFF_VAL_DIM,
    RET_HEADS * RET_KEY_DIM,
    RET_HEADS * RET_KEY_DIM,
    RET_HEADS * RET_VAL_DIM,
    RET_HEADS * RET_VAL_DIM,
    D_MODEL,
    D_MODEL,
)
D_IN = sum(SPLIT_SIZES)

kernel_name = 'hybrid_diffattn_retention_convffn'


def _rms(x, eps):
    xf = x.astype(jnp.float32)
    return xf * lax.rsqrt(jnp.mean(xf * xf, axis=-1, keepdims=True) + eps)


def rms_norm(x, g, eps=NORM_EPS):
    return (_rms(x, eps) * g.astype(jnp.float32)).astype(x.dtype)


def diff_attention(q1, q2, k1, k2, v, lam):
    B, S, H, d = q1.shape
    dv = v.shape[-1]
    nb = S // Q_BLOCK
    scale = d ** -0.5
    slopes = 2.0 ** (-8.0 * (jnp.arange(H, dtype=jnp.float32) + 1.0) / H)
    tk = jnp.arange(S)
    k1f, k2f, vf = k1.astype(jnp.float32), k2.astype(jnp.float32), v.astype(jnp.float32)

    def block(args):
        q1b, q2b, i = args
        tq = i * Q_BLOCK + jnp.arange(Q_BLOCK)
        dist = (tq[:, None] - tk[None, :]).astype(jnp.float32)
        bias = jnp.where(dist >= 0, -slopes[:, None, None] * dist, -jnp.inf)
        s1 = jnp.einsum('bqhd,bkhd->bhqk', q1b.astype(jnp.float32), k1f) * scale + bias
        s2 = jnp.einsum('bqhd,bkhd->bhqk', q2b.astype(jnp.float32), k2f) * scale + bias
        a = jax.nn.softmax(s1, axis=-1) - lam * jax.nn.softmax(s2, axis=-1)
        return jnp.einsum('bhqk,bkhe->bqhe', a, vf)

    q1b = q1.reshape(B, nb, Q_BLOCK, H, d).transpose(1, 0, 2, 3, 4)
    q2b = q2.reshape(B, nb, Q_BLOCK, H, d).transpose(1, 0, 2, 3, 4)
    out = lax.map(block, (q1b, q2b, jnp.arange(nb)))
    return out.transpose(1, 0, 2, 3, 4).reshape(B, S, H, dv)


def retention(q, k, v):
    B, S, H, dk = q.shape
    dv = v.shape[-1]
    C = RET_CHUNK
    nc = S // C
    gammas = 1.0 - 2.0 ** (-5.0 - jnp.arange(H, dtype=jnp.float32))
    log_g = jnp.log(gammas)
    idx = jnp.arange(C, dtype=jnp.float32)
    rel = idx[:, None] - idx[None, :]
    inner_decay = jnp.where(rel >= 0, jnp.exp(log_g[:, None, None] * jnp.maximum(rel, 0.0)), 0.0)
    q_decay = jnp.exp(log_g[:, None] * (idx + 1.0))[..., None]
    k_decay = jnp.exp(log_g[:, None] * (C - 1.0 - idx))[..., None]
    chunk_decay = jnp.exp(log_g * C)[None, :, None, None]

    def to_chunks(t):
        return t.astype(jnp.float32).reshape(B, nc, C, H, -1).transpose(1, 0, 3, 2, 4)

    qc, kc, vc = to_chunks(q), to_chunks(k) * (dk ** -0.5), to_chunks(v)

    def step(state, inp):
        qi, ki, vi = inp
        scores = jnp.einsum('bhqd,bhkd->bhqk', qi, ki) * inner_decay
        o = jnp.einsum('bhqk,bhke->bhqe', scores, vi) + jnp.einsum('bhqd,bhde->bhqe', qi * q_decay, state)
        state = state * chunk_decay + jnp.einsum('bhkd,bhke->bhde', ki * k_decay, vi)
        return state, o

    s0 = jnp.zeros((B, H, dk, dv), jnp.float32)
    _, o = lax.scan(step, s0, (qc, kc, vc))
    return o.transpose(1, 0, 3, 2, 4).reshape(B, S, H, dv)


def causal_dwconv(u, w, b):
    K = w.shape[0]
    S = u.shape[1]
    up = jnp.pad(u, ((0, 0), (K - 1, 0), (0, 0)))
    y = b + up[:, K - 1:K - 1 + S] * w[K - 1]
    for j in range(K - 1):
        y = y + up[:, j:j + S] * w[j]
    return y


def setup_inputs(seed: int = 0) -> dict:
    key = jax.random.key(seed)
    ks = jax.random.split(key, 20)
    L, D, F = DEPTH, D_MODEL, D_FF
    nrm = lambda k, shape, fan: jax.random.normal(k, shape, jnp.float32) * (fan ** -0.5)
    gain = lambda k, shape: 1.0 + 0.02 * jax.random.normal(k, shape, jnp.float32)
    return {
        'x': jax.random.normal(ks[0], (BATCH, SEQ, D), jnp.float32),
        'g_mix': gain(ks[1], (L, D)),
        'w_in': nrm(ks[2], (L, D, D_IN), D),
        'lam_q1': 0.1 * jax.random.normal(ks[3], (L, DIFF_HEAD_DIM), jnp.float32),
        'lam_k1': 0.1 * jax.random.normal(ks[4], (L, DIFF_HEAD_DIM), jnp.float32),
        'lam_q2': 0.1 * jax.random.normal(ks[5], (L, DIFF_HEAD_DIM), jnp.float32),
        'lam_k2': 0.1 * jax.random.normal(ks[6], (L, DIFF_HEAD_DIM), jnp.float32),
        'g_diff_sub': gain(ks[7], (L, DIFF_VAL_DIM)),
        'w_diff_proj': nrm(ks[8], (L, DIFF_HEADS * DIFF_VAL_DIM, D), DIFF_HEADS * DIFF_VAL_DIM),
        'w_ret_proj': nrm(ks[9], (L, RET_HEADS * RET_VAL_DIM, D), RET_HEADS * RET_VAL_DIM),
        'w_out': nrm(ks[10], (L, D, D), D),
        'g_ffn': gain(ks[11], (L, D)),
        'w_up': nrm(ks[12], (L, D, 2 * F), D),
        'conv_w': nrm(ks[13], (L, CONV_WIDTH, 2 * F), CONV_WIDTH),
        'conv_b': 0.01 * jax.random.normal(ks[14], (L, 2 * F), jnp.float32),
        'w_down': nrm(ks[15], (L, F, D), F),
        'g_final': gain(ks[16], (D,)),
    }


def reference(x, g_mix, w_in, lam_q1, lam_k1, lam_q2, lam_k2, g_diff_sub, w_diff_proj, w_ret_proj,
              w_out, g_ffn, w_up, conv_w, conv_b, w_down, g_final):
    B, S, _ = x.shape
    dt = x.dtype
    split_idx = np.cumsum(SPLIT_SIZES)[:-1].tolist()
    for l in range(DEPTH):
        h = rms_norm(x, g_mix[l])
        proj = h @ w_in[l]
        q1, q2, k1, k2, va, qr, kr, vr, gr, gate_a, gate_r = jnp.split(proj, split_idx, axis=-1)

        lam_init = 0.8 - 0.6 * math.exp(-0.3 * l)
        lam = (jnp.exp(jnp.sum(lam_q1[l].astype(jnp.float32) * lam_k1[l].astype(jnp.float32)))
               - jnp.exp(jnp.sum(lam_q2[l].astype(jnp.float32) * lam_k2[l].astype(jnp.float32))) + lam_init)
        a = diff_attention(q1.reshape(B, S, DIFF_HEADS, DIFF_HEAD_DIM), q2.reshape(B, S, DIFF_HEADS, DIFF_HEAD_DIM),
                           k1.reshape(B, S, DIFF_HEADS, DIFF_HEAD_DIM), k2.reshape(B, S, DIFF_HEADS, DIFF_HEAD_DIM),
                           va.reshape(B, S, DIFF_HEADS, DIFF_VAL_DIM), lam)
        a = (_rms(a, SUBLN_EPS) * g_diff_sub[l].astype(jnp.float32) * (1.0 - lam_init)).reshape(B, S, -1).astype(dt)

        r = retention(qr.reshape(B, S, RET_HEADS, RET_KEY_DIM), kr.reshape(B, S, RET_HEADS, RET_KEY_DIM),
                      vr.reshape(B, S, RET_HEADS, RET_VAL_DIM))
        r = (_rms(r, SUBLN_EPS).reshape(B, S, -1) * jax.nn.silu(gr.astype(jnp.float32))).astype(dt)

        merged = jax.nn.sigmoid(gate_a) * (a @ w_diff_proj[l]) + jax.nn.sigmoid(gate_r) * (r @ w_ret_proj[l])
        x = x + merged @ w_out[l]

        h2 = rms_norm(x, g_ffn[l])
        z = causal_dwconv(h2 @ w_up[l], conv_w[l], conv_b[l])
        zg, zu = jnp.split(z, 2, axis=-1)
        x = x + (jax.nn.silu(zg) * zu) @ w_down[l]
    return rms_norm(x, g_final)
```

```python
import math
import numpy as np
import ml_dtypes
import concourse.bass as bass
import concourse.mybir as mybir
from concourse.bass_utils import run_bass_kernel_spmd

F32 = mybir.dt.float32
BF16 = mybir.dt.bfloat16
AF = mybir.ActivationFunctionType
ALU = mybir.AluOpType
AX = mybir.AxisListType

D = 1024
S = 8192
T = 256
H = 4
FF = 2816
NWS = 27
NWD = 11
LAM_INIT = 0.8 - 0.6 * math.exp(-0.3 * 0)
SLOPES = [2.0 ** (-8.0 * (h + 1) / H) for h in range(H)]
GAMMAS = [1.0 - 2.0 ** (-5.0 - h) for h in range(H)]
KWIN = [10, 34, 64, 64]
KOFF = [sum(KWIN[:h]) * 128 for h in range(H)]
VOFF = [sum(KWIN[:h]) * 129 for h in range(H)]
KTOT = sum(KWIN) * 128
VTOT = sum(KWIN) * 129

C_BTAB = 0
C_DT = 256
C_QDEC = 768
C_KDEC = 1024
C_MHALF = 1028
C_GFIN = 1036
C_CW = 2060
NCF = 2060 + 176 * 4
P_GM = 0
P_GF = 8
P_GDS = 16
P_LQ = 17
P_LK = 145
NPF = 273


class Sched:
    def __init__(self):
        self.ops = []
        self.lastw = {}
        self.readers = {}
        self.lastdma = {}

    def add(self, eng, fn, r=(), w=(), dgrp=None):
        oid = len(self.ops)
        deps = set()
        for x in r:
            d = self.lastw.get(x)
            if d is not None:
                deps.add(d)
        for x in w:
            d = self.lastw.get(x)
            if d is not None:
                deps.add(d)
            for d in self.readers.get(x, ()):
                deps.add(d)
        for x in w:
            self.lastw[x] = oid
            self.readers[x] = []
        for x in r:
            if x in w:
                continue
            lst = self.readers.setdefault(x, [])
            if eng != 'sp':
                lst[:] = [o for o in lst if self.ops[o]['eng'] != eng]
            lst.append(oid)
        if eng == 'sp':
            if dgrp in self.lastdma:
                deps.add(self.lastdma[dgrp])
            self.lastdma[dgrp] = oid
        self.ops.append(dict(eng=eng, fn=fn, deps=deps, dgrp=dgrp, sig=False, val=None, tag=getattr(self, 'tag', None)))
        return oid

    def finalize(self):
        for op in self.ops:
            for d in op['deps']:
                dop = self.ops[d]
                if dop['eng'] == 'pe' and op['eng'] == 'pe':
                    continue
                dop['sig'] = True
        cnt = {}
        for op in self.ops:
            if op['eng'] == 'sp':
                g = op['dgrp']
                cnt[g] = cnt.get(g, 0) + 16
                op['val'] = cnt[g]
                op['key'] = g
            else:
                op['key'] = op['eng']
                if op['sig']:
                    cnt[op['eng']] = cnt.get(op['eng'], 0) + 1
                    op['val'] = cnt[op['eng']]
        self.final_counts = cnt

    def emit_engine(self, eng, e, sems):
        known = {}
        for op in self.ops:
            if op['eng'] != eng:
                continue
            need = {}
            for d in op['deps']:
                dop = self.ops[d]
                if dop['eng'] == 'pe' and eng == 'pe':
                    continue
                k = dop['key']
                v = dop['val']
                if v > need.get(k, 0):
                    need[k] = v
            for k, v in need.items():
                if known.get(k, 0) >= v:
                    continue
                e.wait_ge(sems[k], v)
                known[k] = v
            ins = op['fn'](e)
            if eng == 'sp':
                ins.then_inc(sems[op['key']], 16)
            elif op['sig']:
                ins.then_inc(sems[eng], 1)


def build_nc(nblk, stage=99):
    nc = bass.Bass("TRN2", target_bir_lowering=False)
    SL = nblk * T
    x_d = nc.dram_tensor("x", [SL, D], F32, kind="ExternalInput").ap()
    y_d = nc.dram_tensor("y", [SL, D], F32, kind="ExternalOutput").ap()
    wsf = nc.dram_tensor("wsf", [NWS, 128, 4096], F32, kind="ExternalInput").ap()
    wdf = nc.dram_tensor("wdf", [NWD, 128, 2048], F32, kind="ExternalInput").ap()
    cf_d = nc.dram_tensor("cf", [128, NCF], F32, kind="ExternalInput").ap()
    pf_d = nc.dram_tensor("pf", [128, NPF], F32, kind="ExternalInput").ap()
    cb_d = nc.dram_tensor("cb", [128, 256], BF16, kind="ExternalInput").ap()
    wsb = nc.dram_tensor("wsb", [NWS, 128, 4096], BF16).ap()
    wdb = nc.dram_tensor("wdb", [NWD, 128, 2048], BF16).ap()

    sch = Sched()
    A = sch.add

    from contextlib import ExitStack
    es = ExitStack()

    def sb(name, cols, dt):
        return es.enter_context(nc.sbuf_tensor(name, [128, cols], dt))

    kc = sb("kc", KTOT, BF16)
    vc = sb("vc", VTOT, BF16)
    cf = sb("cfs", NCF, F32)
    cb = sb("cbs", 256, BF16)
    st32 = sb("st32", 256, F32)
    stbf = sb("stbf", 256, BF16)
    sm = sb("sm", 64, F32)
    o2t = sb("o2t", 128, F32)
    xs = sb("xs", 2 * 2 * D, F32)
    hb = sb("hb", 8 * 258, BF16)
    NWB = 4
    wb = sb("wb", NWB * 4096, BF16)
    NWD_S = 3
    wd = sb("wd", NWD_S * 2048, BF16)
    qT = sb("qT", 4 * 512, BF16)
    qrT = sb("qrT", 2 * 256, BF16)
    qdT = sb("qdT", 2 * 256, BF16)
    krT = sb("krT", 2 * 256, BF16)
    kd = sb("kd", 2 * 256, BF16)
    vr = sb("vr", 2 * 512, BF16)
    sg = sb("sg", 2 * 512, BF16)
    NPT = 3
    pt = sb("pt", NPT * 512, BF16)
    tmp = sb("tmp", 2048, F32)
    at4 = sb("at4", 1024, F32)
    an = sb("an", 512, BF16)
    aT = sb("aT", 4 * 256, BF16)
    rn = sb("rn", 512, BF16)
    rT = sb("rT", 4 * 256, BF16)
    sd = sb("sd", 2 * 128, BF16)
    mt = sb("mt", 8 * 256, BF16)
    gt = sb("gt", 2 * 512, BF16)
    junk = sb("junk", 128, BF16)

    psall = es.enter_context(nc.psum_tensor("psall", [128, 4096], F32))
    ps = [psall[:, b * 512:(b + 1) * 512] for b in range(8)]
    psb = [p.bitcast(BF16) for p in ps]

    ident = cb[:, 0:128]
    cmask = cb[:, 128:256]
    hb3 = hb[:, :].rearrange("p (k t) -> p k t", k=8)
    xsv = [xs[:, i * 2 * D:(i + 1) * 2 * D].rearrange("p (s d) -> p s d", s=2) for i in range(2)]
    tps = [tmp[:, 0:512], tmp[:, 1024:1536]]
    zzs = [tmp[:, 512:1024], tmp[:, 1536:2048]]
    tp = tps[0]
    zz = zzs[0]
    RES_T = {0: [('at4', 0)], 1: [('at4', 1)]}
    SSQ, MS, RSTD = 0, 2, 4
    NEGLAM = 6
    RL, C2 = 8, 10
    SSA, MSA, RSA = 12, 20, 28
    SSR, MSR, RHR = 36, 44, 52
    LAMT = 60

    SBASE = 0
    ld = [vc[:, SBASE + i * 4096: SBASE + (i + 1) * 4096].bitcast(F32) for i in range(2)]
    cv = [vc[:, SBASE + 8192 + i * 2048: SBASE + 8192 + (i + 1) * 2048] for i in range(2)]
    pfs = vc[:, SBASE + 12288: SBASE + 12288 + 2 * NPF].bitcast(F32)
    lamw = vc[:, SBASE + 12288 + 2 * NPF + 2: SBASE + 12288 + 2 * NPF + 2 + 256].bitcast(F32)

    def mm(out, lhsT, rhs, start, stop, r, w, **kw):
        A('pe', lambda e: e.matmul(out, lhsT, rhs, start=start, stop=stop, **kw), r, w)

    def tr(out, in_, r, w):
        A('pe', lambda e: e.transpose(out, in_, ident), list(r) + ['const'], w)

    def act(out, in_, func, r, w, bias=0.0, scale=1.0, accum_out=None):
        if accum_out is None:
            A('act', lambda e: e.activation(out, in_, func, bias=bias, scale=scale), r, w)
        else:
            A('act', lambda e: e.activation(out, in_, func, bias=bias, scale=scale,
                                             accum_out=accum_out), r, w)

    def ts(eng, out, in0, s1, s2, op0, op1, r, w):
        if op1 is None:
            A(eng, lambda e: e.tensor_scalar(out, in0, s1, None, op0), r, w)
        else:
            A(eng, lambda e: e.tensor_scalar(out, in0, s1, s2, op0, op1), r, w)

    def stt(out, in0, scalar, in1, op0, op1, r, w):
        A('dve', lambda e: e.scalar_tensor_tensor(out, in0, scalar, in1, op0, op1), r, w)

    def tt(eng, out, in0, in1, op, r, w):
        A(eng, lambda e: e.tensor_tensor(out, in0, in1, op), r, w)

    def cp(eng, out, in_, r, w):
        if eng == 'act':
            A(eng, lambda e: e.copy(out, in_), r, w)
        else:
            A(eng, lambda e: e.tensor_copy(out, in_), r, w)

    nr = sb("nr", 32, F32)
    smk = sb("smk", 8, F32)

    def rsqrt(out, x, n, r, w):
        xi = x.bitcast(mybir.dt.int32)
        t1 = nr[:, 0:n].bitcast(mybir.dt.int32)
        y0i = nr[:, 8:8 + n].bitcast(mybir.dt.int32)
        y = nr[:, 8:8 + n]
        a = nr[:, 16:16 + n]
        A('dve', lambda e: e.tensor_scalar(t1, xi, 1, None, ALU.logical_shift_right), list(r), ['nr'])
        A('dve', lambda e: e.tensor_scalar(y0i, t1, -1.0, 1597463007.0, ALU.mult, ALU.add), ['nr'], ['nr'])
        for it in range(3):
            tt('dve', a, x, y, ALU.mult, list(r) + ['nr'], ['nr'])
            tt('dve', a, a, y, ALU.mult, ['nr'], ['nr'])
            ts('dve', a, a, -0.5, 1.5, ALU.mult, ALU.add, ['nr'], ['nr'])
            if it < 2:
                tt('dve', y, y, a, ALU.mult, ['nr'], ['nr'])
            else:
                tt('dve', out, y, a, ALU.mult, ['nr'], list(w) + ['nr'])

    def dma(out, in_, r, w, grp):
        A('sp', lambda e: e.dma_start(out=out, in_=in_), r, w, dgrp=grp)

    dma(cf[:, :], cf_d[:, :], [], ['const'], 'const')
    dma(cb[:, :], cb_d[:, :], [], ['const'], 'const2')
    dma(pfs, pf_d[:, :], [], ['pf'], 'const3')
    A('dve', lambda e: e.memset(st32[:, :], 0.0), [], ['st32'])
    A('dve', lambda e: e.memset(stbf[:, :], 0.0), [], ['stbf'])
    A('dve', lambda e: e.memset(hb[:, :], 0.0), [], ['hb'])
    A('dve', lambda e: e.memset(qT[:, :], 0.0), [], ['qT'])

    tt('dve', lamw, pfs[:, P_LQ:P_LQ + 128], pfs[:, P_LK:P_LK + 128], ALU.mult, ['pf'], ['lamw'])
    A('dve', lambda e: e.reduce_sum(sm[:, LAMT:LAMT + 2],
                                    lamw.rearrange("p (a b) -> p a b", a=2), AX.X),
      ['lamw'], ['lamt'])
    act(sm[:, LAMT:LAMT + 2], sm[:, LAMT:LAMT + 2], AF.Exp, ['lamt'], ['lamt'])
    tt('dve', sm[:, NEGLAM:NEGLAM + 1], sm[:, LAMT + 1:LAMT + 2], sm[:, LAMT:LAMT + 1],
       ALU.subtract, ['lamt'], ['neglam'])
    ts('dve', sm[:, NEGLAM:NEGLAM + 1], sm[:, NEGLAM:NEGLAM + 1], -LAM_INIT, None, ALU.add, None,
       ['neglam'], ['neglam'])

    if stage >= 1:
      pass
    pieces = []
    gm = lambda kt: pfs[:, P_GM + kt:P_GM + kt + 1]
    gf = lambda kt: pfs[:, P_GF + kt:P_GF + kt + 1]
    gds = pfs[:, P_GDS:P_GDS + 1]
    for c in range(NWS):
        for half in range(2):
            if c < 6:
                segs = [(j * 512, (j + 1) * 512, gm(half * 4 + j), None) for j in range(4)]
            elif c < 14:
                if half == 0:
                    segs = [(j * 128, (j + 1) * 128, gm(j % 8), None) for j in range(16)]
                else:
                    segs = [(0, 512, gds, 1.0 - LAM_INIT), (512, 1024, None, None)]
            elif c < 16:
                segs = [(0, 2048, None, None)]
            else:
                segs = [(j * 512, (j + 1) * 512, gf(half * 4 + j), None) for j in range(4)]
            pieces.append((wsf[c, :, half * 2048:(half + 1) * 2048],
                           wsb[c, :, half * 2048:(half + 1) * 2048], segs))
    for c in range(NWD):
        pieces.append((wdf[c, :, :], wdb[c, :, :], [(0, 2048, None, None)]))

    def pload(n):
        i = n % 2
        dma(ld[i], pieces[n][0], [], [('ld', i)], ('ld', i))

    pload(0)
    for n, (src, dst, segs) in enumerate(pieces):
        i = n % 2
        if n + 1 < len(pieces):
            pload(n + 1)
        for (lo, hi, sc, cm) in segs:
            o_ = cv[i][:, lo:hi]
            i_ = ld[i][:, lo:hi]
            if sc is None:
                cp('dve', o_, i_, [('ld', i)], [('cv', i)])
            elif cm is None:
                ts('dve', o_, i_, sc, None, ALU.mult, None, [('ld', i), 'pf'], [('cv', i)])
            else:
                ts('dve', o_, i_, sc, cm, ALU.mult, ALU.mult, [('ld', i), 'pf'], [('cv', i)])
        dma(dst, cv[i], [('cv', i)], [('wsb',)], ('st', i))

    vc_ones = vc[:, :].rearrange("p (n e) -> p n e", e=129)[:, :, 128:129]
    A('dve', lambda e: e.memset(vc_ones, 1.0), [], [('vc', h_, b) for h_ in range(H) for b in range(KWIN[h_])] + [('ld', 0), ('ld', 1), ('cv', 0), ('cv', 1), 'pf', 'lamw'])

    ws_seq = []
    wd_seq = []
    for G in range(nblk):
        ws_seq += list(range(0, 16)) + list(range(16, 27))
        wd_seq += list(range(11))

    class Stream:
        def __init__(self, name, seq, bufs, dram):
            self.name, self.seq, self.bufs, self.dram = name, seq, bufs, dram
            self.nl = 0

        def ensure(self, k):
            while self.nl <= min(k, len(self.seq) - 1):
                i = self.nl
                slot = i % len(self.bufs)
                dma(self.bufs[slot], self.dram[self.seq[i], :, :], [('wsb',)],
                    [(self.name, slot)], (self.name, slot))
                self.nl += 1

        def use(self, k):
            self.ensure(k + len(self.bufs) - 1)
            return k % len(self.bufs)

    wbs = Stream('wb', ws_seq, [wb[:, i * 4096:(i + 1) * 4096] for i in range(NWB)], wsb)
    wds = Stream('wd', wd_seq, [wd[:, i * 2048:(i + 1) * 2048] for i in range(NWD_S)], wdb)
    wk = [0]
    dk = [0]
    bank = [0]

    def nb():
        b = bank[0]
        bank[0] = (b + 1) % 8
        return b

    def rmsnorm_to_hb(G, eps):
        xs3 = xsv[G % 2]
        XS = ('xs', G % 2)
        for s in range(2):
            act(mt[:, s * 1024:(s + 1) * 1024], xs3[:, s, :], AF.Square, [XS], ['mt', 'ssq'],
                accum_out=sm[:, SSQ + s:SSQ + s + 1])
        ts('dve', sm[:, MS:MS + 2], sm[:, SSQ:SSQ + 2], 1.0 / D, eps, ALU.mult, ALU.add, ['ssq'], ['ms'])
        rsqrt(sm[:, RSTD:RSTD + 2], sm[:, MS:MS + 2], 2, ['ms'], ['rstd'])
        for s in range(2):
            act(mt[:, s * 1024:(s + 1) * 1024], xs3[:, s, :], AF.Copy, [XS, 'rstd'], ['mt'],
                scale=sm[:, RSTD + s:RSTD + s + 1])
            b = nb()
            for kt in range(8):
                tr(psb[b][:, kt * 128:(kt + 1) * 128], mt[:, s * 1024 + kt * 128: s * 1024 + (kt + 1) * 128],
                   ['mt'], [('ps', b)])
            cp('dve', hb3[:, :, 2 + s * 128: 2 + (s + 1) * 128],
               psb[b][:, :].rearrange("p (k t) -> p k t", k=8), [('ps', b)], [('ps', b), 'hb'])

    deferred = []
    for G in range(nblk if stage >= 2 else 0):
        sch.tag = (G, 'A')
        xs3 = xsv[G % 2]
        XS = ('xs', G % 2)
        if G == 0:
            dma(xsv[0], x_d[0:T, :].rearrange("(s p) d -> p s d", p=128), [], [('xs', 0)], 'x')
        sch.tag = (G, 'B')
        rmsnorm_to_hb(G, 1e-6)
        if deferred:
            deferred.pop()()
        if G + 1 < nblk:
            dma(xsv[(G + 1) % 2], x_d[(G + 1) * T:(G + 2) * T, :].rearrange("(s p) d -> p s d", p=128), [],
                [('xs', (G + 1) % 2)], 'x')

        if stage <= 2:
            dma(y_d[G * T:(G + 1) * T, :].rearrange("(s p) d -> p s d", p=128), xs3, [XS], ['y'], 'out')
            continue
        sch.tag = (G, 'C')
        def wslot():
            s_ = wbs.use(wk[0])
            wk[0] += 1
            return s_, wb[:, s_ * 4096:(s_ + 1) * 4096], ('wb', s_)

        sl, w_, wr = wslot()
        for h in range(H):
            b = nb()
            for kt in range(8):
                mm(ps[b][:, 0:256], w_[:, kt * 512 + h * 128: kt * 512 + (h + 1) * 128], hb3[:, kt, 2:258],
                   kt == 0, kt == 7, [wr, 'hb'], [('ps', b)])
            cp('dve', qT[0:64, h * 512:h * 512 + 256], ps[b][0:64, 0:256], [('ps', b)], [('ps', b), 'qT'])
            cp('dve', qT[64:128, h * 512 + 256:(h + 1) * 512], ps[b][64:128, 0:256], [('ps', b)], [('ps', b), 'qT'])
        sl, w_, wr = wslot()
        for h in range(H):
            b = nb()
            for kt in range(8):
                mm(ps[b][:, 0:256], w_[:, kt * 512 + h * 128: kt * 512 + (h + 1) * 128], hb3[:, kt, 2:258],
                   kt == 0, kt == 7, [wr, 'hb'], [('ps', b)])
            ks = KOFF[h] + ((2 * G) % KWIN[h]) * 128
            cp('dve', kc[:, ks: ks + 256], ps[b][:, 0:256], [('ps', b)],
               [('ps', b), ('kc', h, ((2 * G) % KWIN[h]) // 2)])
        sl, w_, wr = wslot()
        for s in range(2):
            b = nb()
            blk = 2 * G + s
            for kt in range(8):
                mm(ps[b][:, 0:512], hb3[:, kt, 2 + s * 128: 2 + (s + 1) * 128], w_[:, kt * 512:(kt + 1) * 512],
                   kt == 0, kt == 7, [wr, 'hb'], [('ps', b)])
            for h in range(H):
                vs = VOFF[h] + (blk % KWIN[h]) * 129
                cp('dve', vc[:, vs: vs + 128], ps[b][:, h * 128:(h + 1) * 128], [('ps', b)],
                   [('ps', b), ('vc', h, blk % KWIN[h])])
        sl, w_, wr = wslot()
        for i in range(2):
            b = nb()
            for kt in range(8):
                mm(ps[b][:, 0:256], w_[:, kt * 512 + i * 128: kt * 512 + (i + 1) * 128], hb3[:, kt, 2:258],
                   kt == 0, kt == 7, [wr, 'hb'], [('ps', b)])
            cp('dve', qrT[:, i * 256:(i + 1) * 256], ps[b][:, 0:256], [('ps', b)], [('ps', b), 'qrT'])
            for s in range(2):
                tt('dve', qdT[:, i * 256 + s * 128: i * 256 + (s + 1) * 128], ps[b][:, s * 128:(s + 1) * 128],
                   cf[:, C_QDEC + i * 128: C_QDEC + (i + 1) * 128], ALU.mult,
                   [('ps', b), 'const'], [('ps', b), 'qdT'])
        for i in range(2):
            b = nb()
            for kt in range(8):
                mm(ps[b][:, 0:256], w_[:, kt * 512 + 256 + i * 128: kt * 512 + 256 + (i + 1) * 128],
                   hb3[:, kt, 2:258], kt == 0, kt == 7, [wr, 'hb'], [('ps', b)])
            cp('dve', krT[:, i * 256:(i + 1) * 256], ps[b][:, 0:256], [('ps', b)], [('ps', b), 'krT'])
        for s in range(2):
            b = nb()
            for kt in range(8):
                mm(ps[b][:, 0:256], hb3[:, kt, 2 + s * 128: 2 + (s + 1) * 128],
                   w_[:, kt * 512 + 256: kt * 512 + 512], kt == 0, kt == 7, [wr, 'hb'], [('ps', b)])
            for h in range(H):
                ts('dve', kd[:, s * 256 + h * 64: s * 256 + (h + 1) * 64], ps[b][:, h * 64:(h + 1) * 64],
                   cf[:, C_KDEC + h:C_KDEC + h + 1], None, ALU.mult, None,
                   [('ps', b), 'const'], [('ps', b), 'kd'])
        sl, w_, wr = wslot()
        for s in range(2):
            b = nb()
            for kt in range(8):
                mm(ps[b][:, 0:512], hb3[:, kt, 2 + s * 128: 2 + (s + 1) * 128], w_[:, kt * 512:(kt + 1) * 512],
                   kt == 0, kt == 7, [wr, 'hb'], [('ps', b)])
            cp('dve', vr[:, s * 512:(s + 1) * 512], ps[b][:, 0:512], [('ps', b)], [('ps', b), 'vr'])
        sl, w_, wr = wslot()
        for s in range(2):
            b = nb()
            for kt in range(8):
                mm(ps[b][:, 0:512], hb3[:, kt, 2 + s * 128: 2 + (s + 1) * 128], w_[:, kt * 512:(kt + 1) * 512],
                   kt == 0, kt == 7, [wr, 'hb'], [('ps', b)])
            act(tps[s], ps[b][:, 0:512], AF.Tanh, [('ps', b)], [('ps', b), ('tp', s), ('tp2', s)], scale=0.5)
            stt(sg[:, s * 512:(s + 1) * 512], tps[s], 1.0, ps[b][:, 0:512], ALU.add, ALU.mult,
                [('tp', s), ('tp2', s), ('ps', b)], [('ps', b), 'sg'])

        if stage <= 3:
            dma(y_d[G * T:(G + 1) * T, :].rearrange("(s p) d -> p s d", p=128), xs3, [XS], ['y'], 'out')
            continue
        sch.tag = (G, 'D')
        units = []
        for h in range(H):
            for j in range(max(0, 2 * G - (KWIN[h] - 2)), 2 * G + 2):
                units.append((h, j))
        firstflag = {}

        def a_qk(i):
            h, j = units[i]
            sb_ = i % 4
            diag_qb = j - 2 * G
            isd = diag_qb >= 0
            ksl = j % KWIN[h]
            mm(ps[sb_][:, 0:512], kc[:, KOFF[h] + ksl * 128: KOFF[h] + (ksl + 1) * 128],
               qT[:, h * 512:(h + 1) * 512], True, not isd, [('kc', h, ksl // 2), 'qT'], [('ps', sb_)])
            if isd:
                for m in range(2):
                    mm(ps[sb_][:, m * 256 + diag_qb * 128: m * 256 + (diag_qb + 1) * 128],
                       ident, cmask, False, m == 1, ['const'], [('ps', sb_)])

        def a_exp(i):
            h, j = units[i]
            c0 = 0 if j <= 2 * G else 128
            sb_ = i % 4
            pi = i % NPT
            sres = [('ps', sb_)]
            ptb = pt[:, pi * 512:(pi + 1) * 512]
            qbs = [qb for qb in range(2) if qb * 128 >= c0]
            pv3 = ps[sb_][:, 0:512].rearrange("p (m q) -> p m q", m=2)
            pt3 = ptb.rearrange("p (m q) -> p m q", m=2)
            if h == 0:
                for qb in qbs:
                    o = j - (2 * G + qb)
                    act(pt3[:, :, qb * 128:(qb + 1) * 128], pv3[:, :, qb * 128:(qb + 1) * 128], AF.Exp,
                        sres + ['const'], sres + [('pt', pi)],
                        bias=cf[:, C_BTAB + h * 64 + o + 63: C_BTAB + h * 64 + o + 64], scale=0.125)
            else:
                o = j - (2 * G + 1)
                act(pt3[:, :, c0:256], pv3[:, :, c0:256], AF.Exp,
                    sres + ['const'], sres + [('pt', pi)],
                    bias=cf[:, C_BTAB + h * 64 + o + 63: C_BTAB + h * 64 + o + 64], scale=0.125)

        def a_pv(i):
            h, j = units[i]
            c0 = 0 if j <= 2 * G else 128
            pi = i % NPT
            ptb = pt[:, pi * 512:(pi + 1) * 512]
            vsl = j % KWIN[h]
            for qb in [qb for qb in range(2) if qb * 128 >= c0]:
                ob = 4 + (h % 2) * 2 + qb
                lastj = 2 * G + qb
                for m in range(2):
                    mm(ps[ob][:, m * 129:(m + 1) * 129],
                       ptb[:, m * 256 + qb * 128: m * 256 + (qb + 1) * 128],
                       vc[:, VOFF[h] + vsl * 129: VOFF[h] + (vsl + 1) * 129],
                       firstflag.get((h, qb), True), (j == lastj), [('pt', pi), ('vc', h, vsl)], [('ps', ob)],
                       skip_group_check=True)
                    firstflag[(h, qb)] = False

        def a_epi(h):
            for qb in range(2):
                ob = 4 + (h % 2) * 2 + qb
                o3 = ps[ob][:, 0:258].rearrange("p (m e) -> p m e", m=2)
                rl3 = sm[:, RL:RL + 2].rearrange("p (m e) -> p m e", m=2)
                A('dve', lambda e, o3=o3, rl3=rl3: e.reciprocal(rl3, o3[:, :, 128:129]),
                  [('ps', ob)], [('ps', ob), 'rl'])
                ts('dve', sm[:, C2:C2 + 1], sm[:, RL + 1:RL + 2], sm[:, NEGLAM:NEGLAM + 1], None, ALU.mult, None,
                   ['rl', 'neglam'], ['c2'])
                ts('dve', o2t[:, :], ps[ob][:, 129:257], sm[:, C2:C2 + 1], None, ALU.mult, None,
                   [('ps', ob), 'c2'], [('ps', ob), 'o2t'])
                at = at4[:, qb * 512 + h * 128: qb * 512 + (h + 1) * 128]
                stt(at, ps[ob][:, 0:128], sm[:, RL:RL + 1], o2t[:, :], ALU.mult, ALU.add,
                    [('ps', ob), 'rl', 'o2t'], [('ps', ob)] + RES_T[qb])
                acc = sm[:, SSA + qb * 4 + h: SSA + qb * 4 + h + 1]
                A('dve', lambda e, at=at, acc=acc: e.scalar_tensor_tensor(o2t[:, :], at, 1.0, at, ALU.mult, ALU.mult,
                                                                           accum_out=acc),
                  RES_T[qb], ['o2t', 'ssa'])

        for i0 in range(min(2, len(units))):
            a_qk(i0)
        for i in range(len(units)):
            if i + 2 < len(units):
                a_qk(i + 2)
            a_exp(i)
            a_pv(i)
            if i + 1 == len(units) or units[i + 1][0] != units[i][0]:
                a_epi(units[i][0])
        ts('dve', sm[:, MSA:MSA + 8], sm[:, SSA:SSA + 8], 1.0 / 128, 1e-5, ALU.mult, ALU.add, ['ssa'], ['msa'])
        rsqrt(sm[:, RSA:RSA + 8], sm[:, MSA:MSA + 8], 8, ['msa'], ['rsa'])
        for qb in range(2):
            for h in range(H):
                ts('dve', an[:, h * 128:(h + 1) * 128], at4[:, qb * 512 + h * 128: qb * 512 + (h + 1) * 128],
                   sm[:, RSA + qb * 4 + h: RSA + qb * 4 + h + 1], None, ALU.mult, None,
                   RES_T[qb] + ['rsa'], ['an'])
            b = nb()
            for h in range(H):
                tr(psb[b][:, h * 128:(h + 1) * 128], an[:, h * 128:(h + 1) * 128], ['an'], [('ps', b)])
            cp('dve', aT[:, :].rearrange("p (h t) -> p h t", h=4)[:, :, qb * 128:(qb + 1) * 128],
               psb[b][:, 0:512].rearrange("p (h t) -> p h t", h=4), [('ps', b)], [('ps', b), 'aT'])

        if stage <= 4:
            dma(y_d[G * T:(G + 1) * T, :].rearrange("(s p) d -> p s d", p=128), xs3, [XS], ['y'], 'out')
            continue
        sch.tag = (G, 'E')
        for s in range(2):
            bo = nb()
            for h in range(H):
                i, hf = h // 2, h % 2
                bs = nb()
                if bs == bo:
                    bs = nb()
                pr_ = slice(hf * 64, (hf + 1) * 64)
                cs = slice(i * 256 + s * 128, i * 256 + (s + 1) * 128)
                mm(ps[bs][:, 0:128], krT[pr_, cs], qrT[pr_, cs], True, True, ['krT', 'qrT'], [('ps', bs)])
                sdi = sd[:, (h % 2) * 128:(h % 2 + 1) * 128]
                tt('dve', sdi, ps[bs][:, 0:128], cf[:, C_DT + h * 128: C_DT + (h + 1) * 128], ALU.mult,
                   [('ps', bs), 'const'], [('ps', bs), ('sd', h % 2)])
                mm(ps[bo][:, h * 128:(h + 1) * 128], sdi, vr[:, s * 512 + h * 128: s * 512 + (h + 1) * 128],
                   True, False, [('sd', h % 2), 'vr'], [('ps', bo)])
                mm(ps[bo][:, h * 128:(h + 1) * 128], qdT[pr_, cs], stbf[pr_, i * 128:(i + 1) * 128],
                   False, True, ['qdT', 'stbf'], [('ps', bo)])
            for i in range(2):
                for hf in range(2):
                    h = 2 * i + hf
                    bu = nb()
                    if bu == bo:
                        bu = nb()
                    mm(ps[bu][:, 0:128], kd[:, s * 256 + i * 128: s * 256 + (i + 1) * 128],
                       vr[:, s * 512 + h * 128: s * 512 + (h + 1) * 128], True, True, ['kd', 'vr'], [('ps', bu)])
                    pr_ = slice(hf * 64, (hf + 1) * 64)
                    stt(st32[pr_, i * 128:(i + 1) * 128], st32[pr_, i * 128:(i + 1) * 128],
                        float(GAMMAS[h] ** 128), ps[bu][pr_, 0:128], ALU.mult, ALU.add,
                        [('ps', bu), 'st32'], [('ps', bu), 'st32'])
                cp('dve', stbf[:, i * 128:(i + 1) * 128], st32[:, i * 128:(i + 1) * 128], ['st32'], ['stbf'])
            for h in range(H):
                act(junk[:, :], ps[bo][:, h * 128:(h + 1) * 128], AF.Square, [('ps', bo)],
                    [('ps', bo), 'junk', 'ssr'], accum_out=sm[:, SSR + h:SSR + h + 1])
            ts('dve', sm[:, MSR:MSR + 4], sm[:, SSR:SSR + 4], 4.0 / 128, 4e-5, ALU.mult, ALU.add, ['ssr'], ['msr'])
            rsqrt(sm[:, RHR:RHR + 4], sm[:, MSR:MSR + 4], 4, ['msr'], ['rhr'])
            for h in range(H):
                stt(rn[:, h * 128:(h + 1) * 128], ps[bo][:, h * 128:(h + 1) * 128], sm[:, RHR + h:RHR + h + 1],
                    sg[:, s * 512 + h * 128: s * 512 + (h + 1) * 128], ALU.mult, ALU.mult,
                    [('ps', bo), 'rhr', 'sg'], [('ps', bo), 'rn'])
            b = nb()
            for h in range(H):
                tr(psb[b][:, h * 128:(h + 1) * 128], rn[:, h * 128:(h + 1) * 128], ['rn'], [('ps', b)])
            cp('dve', rT[:, :].rearrange("p (h t) -> p h t", h=4)[:, :, s * 128:(s + 1) * 128],
               psb[b][:, 0:512].rearrange("p (h t) -> p h t", h=4), [('ps', b)], [('ps', b), 'rT'])

        if stage <= 5:
            dma(y_d[G * T:(G + 1) * T, :].rearrange("(s p) d -> p s d", p=128), xs3, [XS], ['y'], 'out')
            continue
        sch.tag = (G, 'F')
        for m in range(8):
            sl, w_, wr = wslot()
            wm = w_.rearrange("p (k c) -> p k c", c=128)
            bg = nb()
            bp = nb()
            for kt in range(8):
                mm(ps[bg][:, 0:256], wm[:, kt, :], hb3[:, kt, 2:258], kt == 0, kt == 7, [wr, 'hb'], [('ps', bg)])
            for kt in range(8):
                mm(ps[bg][:, 256:512], wm[:, 8 + kt, :], hb3[:, kt, 2:258], kt == 0, kt == 7, [wr, 'hb'],
                   [('ps', bg)])
            for kt in range(4):
                mm(ps[bp][:, 0:256], wm[:, 16 + kt, :], aT[:, kt * 256:(kt + 1) * 256], kt == 0, kt == 3,
                   [wr, 'aT'], [('ps', bp)])
            for kt in range(4):
                mm(ps[bp][:, 256:512], wm[:, 20 + kt, :], rT[:, kt * 256:(kt + 1) * 256], kt == 0, kt == 3,
                   [wr, 'rT'], [('ps', bp)])
            tpm, zzm, ti = tps[m % 2], zzs[m % 2], m % 2
            act(tpm, ps[bg][:, 0:512], AF.Tanh, [('ps', bg)], [('ps', bg), ('tp', ti), ('tp2', ti)], scale=0.5)
            stt(zzm, tpm, 1.0, ps[bp][:, 0:512], ALU.add, ALU.mult, [('tp', ti), ('tp2', ti), ('ps', bp)],
                [('ps', bp), ('zz', ti, 0), ('zz', ti, 1)])
            tt('dve', mt[:, m * 256:(m + 1) * 256], zzm[:, 0:256], zzm[:, 256:512], ALU.add, [('zz', ti, 0), ('zz', ti, 1)], ['mt'])

        if stage <= 6:
            dma(y_d[G * T:(G + 1) * T, :].rearrange("(s p) d -> p s d", p=128), xs3, [XS], ['y'], 'out')
            continue
        sch.tag = (G, 'G')
        for half in range(2):
            sl, w_, wr = wslot()
            for s in range(2):
                b = nb()
                for kt in range(8):
                    mm(ps[b][:, 0:512], mt[:, kt * 256 + s * 128: kt * 256 + (s + 1) * 128],
                       w_[:, kt * 512:(kt + 1) * 512], kt == 0, kt == 7, [wr, 'mt'], [('ps', b)])
                stt(xs3[:, s, half * 512:(half + 1) * 512], ps[b][:, 0:512], 0.5,
                    xs3[:, s, half * 512:(half + 1) * 512], ALU.mult, ALU.add,
                    [('ps', b), XS], [('ps', b), XS])

        if stage <= 7:
            dma(y_d[G * T:(G + 1) * T, :].rearrange("(s p) d -> p s d", p=128), xs3, [XS], ['y'], 'out')
            continue
        sch.tag = (G, 'H')
        rmsnorm_to_hb(G, 1e-6)

        sch.tag = (G, 'I')
        bank[0] = 0
        ffw = {}

        def f_u(k):
            c, e_ = k // 2, k % 2
            if c not in ffw:
                ffw[c] = wslot()
            sl, w_, wr = ffw[c]
            for which in range(2):
                t_ = which * 2 + e_
                ub = (e_ * 2 + which)
                for kt in range(8):
                    mm(ps[ub][:, 0:258], w_[:, kt * 512 + t_ * 128: kt * 512 + (t_ + 1) * 128],
                       hb3[:, kt, 0:258], kt == 0, kt == 7, [wr, 'hb'], [('ps', ub)])

        def f_elem(k):
            c, e_ = k // 2, k % 2
            gi = c % 2
            gtb = gt[:, gi * 512:(gi + 1) * 512]
            for which in range(2):
                t_ = which * 2 + e_
                ub = (e_ * 2 + which)
                cwb = C_CW + (c * 4 + t_) * 4
                zt = zzs[e_][:, which * 256:(which + 1) * 256]
                ZR = ('zz', e_, which)
                act(zt, ps[ub][:, 2:258], AF.Identity, [('ps', ub), 'const'], [('ps', ub), ZR],
                    bias=cf[:, cwb + 3:cwb + 4], scale=cf[:, cwb + 2:cwb + 3])
                stt(zt, ps[ub][:, 1:257], cf[:, cwb + 1:cwb + 2], zt, ALU.mult, ALU.add,
                    [('ps', ub), 'const', ZR], [('ps', ub), ZR])
                stt(zt, ps[ub][:, 0:256], cf[:, cwb:cwb + 1], zt, ALU.mult, ALU.add,
                    [('ps', ub), 'const', ZR], [('ps', ub), ZR])
            tpe, zze = tps[e_], zzs[e_]
            act(tpe[:, 0:256], zze[:, 0:256], AF.Tanh, [('zz', e_, 0)], [('tp', e_)], scale=0.5)
            tt('dve', tpe[:, 256:512], zze[:, 0:256], zze[:, 256:512], ALU.mult, [('zz', e_, 0), ('zz', e_, 1)],
               [('tp2', e_)])
            stt(gtb[:, e_ * 256:(e_ + 1) * 256], tpe[:, 0:256], 1.0, tpe[:, 256:512], ALU.add, ALU.mult,
                [('tp', e_), ('tp2', e_)], [('gt', gi, e_)])

        def f_down(k):
            c, e_ = k // 2, k % 2
            gi = c % 2
            gtb = gt[:, gi * 512:(gi + 1) * 512]
            if e_ == 0:
                dsl = wds.use(dk[0])
                dk[0] += 1
                ffw[('d', c)] = dsl
            dsl = ffw[('d', c)]
            wd_ = wd[:, dsl * 2048:(dsl + 1) * 2048]
            for s in range(2):
                for half in range(2):
                    yb = 4 + s * 2 + half
                    mm(ps[yb][:, 0:512], gtb[:, e_ * 256 + s * 128: e_ * 256 + (s + 1) * 128],
                       wd_[:, e_ * 1024 + half * 512: e_ * 1024 + (half + 1) * 512],
                       (c == 0 and e_ == 0), (c == 10 and e_ == 1), [('gt', gi, e_), ('wd', dsl)], [('ps', yb)])

        f_u(0)
        for k in range(22):
            if k + 1 < 22:
                f_u(k + 1)
            f_elem(k)
            f_down(k)
        cp('dve', hb3[:, :, 0:2], hb3[:, :, 256:258], ['hb'], ['hb'])
        for s in range(2):
            for half in range(2):
                yb = 4 + s * 2 + half
                stt(xs3[:, s, half * 512:(half + 1) * 512], ps[yb][:, 0:512], 0.5,
                    xs3[:, s, half * 512:(half + 1) * 512], ALU.mult, ALU.add,
                    [('ps', yb), XS], [('ps', yb), XS])

        if stage <= 8:
            dma(y_d[G * T:(G + 1) * T, :].rearrange("(s p) d -> p s d", p=128), xs3, [XS], ['y'], 'out')
            continue
        def emit_K(G=G, xs3=xs3, XS=XS):
            sch.tag = (G, 'K')
            jk = tps[0].bitcast(BF16)
            for s in range(2):
                act(jk, xs3[:, s, :], AF.Square, [XS], [('tp', 0), ('tp2', 0), 'ssqk'],
                    accum_out=smk[:, s:s + 1])
            ts('dve', smk[:, 2:4], smk[:, 0:2], 1.0 / D, 1e-6, ALU.mult, ALU.add, ['ssqk'], ['msk'])
            rsqrt(smk[:, 4:6], smk[:, 2:4], 2, ['msk'], ['rstdk'])
            for s in range(2):
                stt(xs3[:, s, :], xs3[:, s, :], smk[:, 4 + s:5 + s], cf[:, C_GFIN:C_GFIN + D],
                    ALU.mult, ALU.mult, [XS, 'rstdk', 'const'], [XS])
            dma(y_d[G * T:(G + 1) * T, :].rearrange("(s p) d -> p s d", p=128), xs3, [XS], ['y'], 'out')
        deferred.append(emit_K)

    while deferred:
        deferred.pop()()

    sch.finalize()
    global _LAST_SCHED
    _LAST_SCHED = sch
    keys = ['pe', 'act', 'dve'] + sorted({op['dgrp'] for op in sch.ops if op['eng'] == 'sp'}, key=str)
    sems = {}
    for k in keys:
        sems[k] = es.enter_context(nc.semaphore("s_" + str(k).replace(" ", "").replace("'", "")
                                                .replace("(", "").replace(")", "").replace(",", "_")))
    with nc.allow_low_precision("bf16 matmul operands, fp32 accumulation"):
        with nc.Block() as block:
            @block.tensor
            def _(e):
                sch.emit_engine('pe', e, sems)

            @block.scalar
            def _(e):
                sch.emit_engine('act', e, sems)

            @block.vector
            def _(e):
                sch.emit_engine('dve', e, sems)

            @block.sync
            def _(e):
                sch.emit_engine('sp', e, sems)
                if 'out' in sems:
                    e.wait_ge(sems['out'], sch.final_counts['out'])
    es.close()
    return nc


def _tile_k(w, cols):
    K = w.shape[0]
    sub = w[:, cols]
    return np.ascontiguousarray(sub.reshape(K // 128, 128, len(cols)).transpose(1, 0, 2))


def _host_weights(w_in, w_dp, w_rp, w_out, w_up, w_down):
    wsf = np.zeros((NWS, 128, 4096), np.float32)
    ar = np.arange
    q1, q2, k1, k2 = 0, 256, 512, 768
    vo, qro, kro, vro, gro, gao, gro2 = 1024, 1536, 1792, 2048, 2560, 3072, 4096
    cols0 = np.concatenate([np.concatenate([q1 + h * 64 + ar(64), q2 + h * 64 + ar(64)]) for h in range(4)])
    cols1 = np.concatenate([np.concatenate([k1 + h * 64 + ar(64), k2 + h * 64 + ar(64)]) for h in range(4)])
    chunks = [cols0, cols1, vo + ar(512), np.concatenate([qro + ar(256), kro + ar(256)]), vro + ar(512),
              gro + ar(512)]
    for c, cols in enumerate(chunks):
        wsf[c] = _tile_k(w_in, cols).reshape(128, 4096)
    for m in range(8):
        ga = _tile_k(w_in, gao + m * 128 + ar(128))
        gr = _tile_k(w_in, gro2 + m * 128 + ar(128))
        dp = _tile_k(w_dp, m * 128 + ar(128))
        rp = _tile_k(w_rp, m * 128 + ar(128))
        blk = np.concatenate([ga, gr, dp, rp], axis=1).reshape(128, 24 * 128)
        wsf[6 + m, :, :3072] = blk
    for half in range(2):
        wsf[14 + half] = _tile_k(w_out, half * 512 + ar(512)).reshape(128, 4096)
    for c in range(11):
        cols = np.concatenate([(2 * c) * 128 + ar(128), (2 * c + 1) * 128 + ar(128),
                               FF + (2 * c) * 128 + ar(128), FF + (2 * c + 1) * 128 + ar(128)])
        wsf[16 + c] = _tile_k(w_up, cols).reshape(128, 4096)
    wdf = np.zeros((NWD, 128, 2048), np.float32)
    for c in range(11):
        rows = w_down[2 * c * 128:(2 * c + 2) * 128, :]
        wdf[c] = rows.reshape(2, 128, 1024).transpose(1, 0, 2).reshape(128, 2048)
    return wsf, wdf


def _host_consts(g_final, conv_w, conv_b):
    cf = np.zeros((128, NCF), np.float32)
    p = np.arange(128, dtype=np.float64)
    for h in range(4):
        for oi in range(64):
            o = oi - 63
            cf[:, C_BTAB + h * 64 + oi] = SLOPES[h] * (p - 127.0 + 128.0 * o)
        g = GAMMAS[h]
        kk = p[:, None]
        qq = p[None, :]
        rel = qq - kk
        dmat = np.where(rel >= 0, np.exp(np.log(g) * np.maximum(rel, 0.0)), 0.0) * (64 ** -0.5)
        cf[:, C_DT + h * 128: C_DT + (h + 1) * 128] = dmat
        cf[:, C_KDEC + h] = np.exp(np.log(g) * (127.0 - p)) * (64 ** -0.5)
    for i in range(2):
        for hf in range(2):
            g = GAMMAS[2 * i + hf]
            cf[hf * 64:(hf + 1) * 64, C_QDEC + i * 128: C_QDEC + (i + 1) * 128] = \
                np.exp(np.log(g) * (p + 1.0))[None, :]
    cf[:, C_MHALF:C_MHALF + 8] = -0.5
    cf[:, C_GFIN:C_GFIN + D] = g_final[None, :]
    for c in range(11):
        for t_ in range(4):
            which, e_ = t_ // 2, t_ % 2
            ch = which * FF + (2 * c + e_) * 128 + np.arange(128)
            base = C_CW + (c * 4 + t_) * 4
            for j in range(3):
                cf[:, base + j] = conv_w[j, ch]
            cf[:, base + 3] = conv_b[ch]
    cb = np.zeros((128, 256), np.float32)
    cb[:, 0:128] = np.eye(128, dtype=np.float32)
    kk = np.arange(128)[:, None]
    qq = np.arange(128)[None, :]
    cb[:, 128:256] = np.where(kk > qq, -30000.0, 0.0)
    return cf, cb.astype(ml_dtypes.bfloat16)


def _host_pf(g_mix, g_ffn, g_ds, lq1, lk1, lq2, lk2):
    pf = np.zeros((128, NPF), np.float32)
    pf[:, P_GM:P_GM + 8] = g_mix.reshape(8, 128).T
    pf[:, P_GF:P_GF + 8] = g_ffn.reshape(8, 128).T
    pf[:, P_GDS] = g_ds
    pf[:, P_LQ:P_LQ + 128] = np.concatenate([lq1, lq2])[None, :]
    pf[:, P_LK:P_LK + 128] = np.concatenate([lk1, lk2])[None, :]
    return pf


_NC_CACHE = {}


def _run(inputs, nblk, ncores, stage=99):
    f = lambda k: np.asarray(inputs[k], dtype=np.float32)
    x = f('x')
    wsf, wdf = _host_weights(f('w_in')[0], f('w_diff_proj')[0], f('w_ret_proj')[0], f('w_out')[0],
                             f('w_up')[0], f('w_down')[0])
    cf, cb = _host_consts(f('g_final'), f('conv_w')[0], f('conv_b')[0])
    pf = _host_pf(f('g_mix')[0], f('g_ffn')[0], f('g_diff_sub')[0], f('lam_q1')[0], f('lam_k1')[0],
                  f('lam_q2')[0], f('lam_k2')[0])
    if (nblk, stage) not in _NC_CACHE:
        _NC_CACHE[(nblk, stage)] = build_nc(nblk, stage)
    nc = _NC_CACHE[(nblk, stage)]
    SL = nblk * T
    in_maps = []
    for b in range(ncores):
        in_maps.append({"x": np.ascontiguousarray(x[b, :SL, :]), "wsf": wsf, "wdf": wdf, "cf": cf, "pf": pf,
                        "cb": cb})
    res = run_bass_kernel_spmd(nc, in_maps, core_ids=list(range(ncores)))
    return np.stack([np.asarray(r["y"], dtype=np.float32) for r in res.results], axis=0)


def kernel(**inputs):
    return _run(inputs, S // T, 8)
```
